# Optimizing a Trainium2 kernel written in Bass

```python
import jax, jax.numpy as jnp
from jax import lax
import numpy as np

D_MODEL = 4096
BATCH = 1
SEQ = 8192
DEPTH = 1

MIX_WIDTH = D_MODEL
HEAD_DIM = 128
SB_WIDTH = MIX_WIDTH // 2
SB_HEADS = SB_WIDTH // HEAD_DIM
SGU_WIDTH = MIX_WIDTH - SB_WIDTH
SGU_GROUP = 128
SGU_GROUPS = SGU_WIDTH // SGU_GROUP
CHUNK = 128
Q_BLOCK = 128
IN_COLS = 3 * SB_WIDTH + 2 * SGU_WIDTH
PEER_HEADS = 8
PEER_NKEYS = 128
PEER_EXPERTS = PEER_NKEYS * PEER_NKEYS
PEER_QDIM = 512
PEER_HALF = PEER_QDIM // 2
PEER_TOPK = 16
PEER_TOK_CHUNK = 64
EPS = 1e-6

kernel_name = "hymba_style_stickbreak_sgu_peer"


def rms_norm(x, g):
    xf = x.astype(jnp.float32)
    y = xf * lax.rsqrt(jnp.mean(xf * xf, axis=-1, keepdims=True) + EPS)
    return (y * g.astype(jnp.float32)).astype(x.dtype)


def stick_breaking_attention(q, k, v):
    B, H, S, Dh = q.shape
    nb = S // Q_BLOCK
    qb = q.reshape(B, H, nb, Q_BLOCK, Dh).transpose(2, 0, 1, 3, 4)
    starts = jnp.arange(nb, dtype=jnp.int32) * Q_BLOCK
    kpos = jnp.arange(S, dtype=jnp.int32)
    kf = k.astype(jnp.float32)
    vf = v.astype(jnp.float32)
    scale = HEAD_DIM ** -0.5

    def block(args):
        qblk, start = args
        z = jnp.einsum('bhqd,bhkd->bhqk', qblk.astype(jnp.float32), kf) * scale
        qpos = start + jnp.arange(Q_BLOCK, dtype=jnp.int32)
        mask = kpos[None, :] < qpos[:, None]
        log_1m = jnp.where(mask, jax.nn.log_sigmoid(-z), 0.0)
        suffix = lax.cumsum(log_1m, axis=3, reverse=True) - log_1m
        a = jnp.where(mask, jnp.exp(jax.nn.log_sigmoid(z) + suffix), 0.0)
        return jnp.einsum('bhqk,bhkd->bhqd', a, vf)

    out = lax.map(block, (qb, starts))
    return out.transpose(1, 2, 0, 3, 4).reshape(B, H, S, Dh)


def spatial_gating(u, v, v_norm_g, w_s, b_s):
    B, S, _ = u.shape
    nc = S // CHUNK
    v5 = v.reshape(B, nc, CHUNK, SGU_GROUPS, SGU_GROUP)
    vn = rms_norm(v5, v_norm_g.reshape(SGU_GROUPS, SGU_GROUP))
    w_causal = jnp.tril(w_s)
    mixed = jnp.einsum('gts,bnsgc->bntgc', w_causal, vn) + b_s.T[:, :, None]
    out = u.reshape(B, nc, CHUNK, SGU_GROUPS, SGU_GROUP) * mixed
    return out.reshape(B, S, SGU_WIDTH).astype(u.dtype)


def peer_ffn(h, w_q, sub_keys, u_tab, v_tab):
    B, S, D = h.shape
    T = B * S
    K = PEER_TOPK
    t = h.reshape(T, D)
    q = (t @ w_q).reshape(T, PEER_HEADS, 2, PEER_HALF)
    scores = jnp.einsum('thpd,hpnd->thpn', q, sub_keys).astype(jnp.float32)
    s_top, i_top = lax.top_k(scores, K)
    cand = (s_top[:, :, 0, :, None] + s_top[:, :, 1, None, :]).reshape(T, PEER_HEADS, K * K)
    best, flat = lax.top_k(cand, K)
    i1 = jnp.take_along_axis(i_top[:, :, 0], flat // K, axis=-1)
    i2 = jnp.take_along_axis(i_top[:, :, 1], flat % K, axis=-1)
    expert = (i1 * PEER_NKEYS + i2).reshape(T, PEER_HEADS * K)
    gate = jax.nn.softmax(best, axis=-1).reshape(T, PEER_HEADS * K)
    nt = T // PEER_TOK_CHUNK

    def chunk(args):
        tc, ec, gc = args
        uc = u_tab[ec]
        act = jax.nn.gelu(jnp.einsum('cd,ced->ce', tc, uc).astype(jnp.float32))
        coef = (gc * act).astype(tc.dtype)
        return jnp.einsum('ce,ced->cd', coef, v_tab[ec])

    out = lax.map(chunk, (t.reshape(nt, PEER_TOK_CHUNK, D),
                          expert.reshape(nt, PEER_TOK_CHUNK, PEER_HEADS * K),
                          gate.reshape(nt, PEER_TOK_CHUNK, PEER_HEADS * K)))
    return out.reshape(B, S, D).astype(h.dtype)


def setup_inputs(seed: int = 0) -> dict:
    key = jax.random.key(seed)
    ks = jax.random.split(key, 16)
    f32 = jnp.float32
    nrm = lambda k, shape, s: jax.random.normal(k, shape, f32) * s
    gain = lambda k, shape: 1.0 + 0.02 * jax.random.normal(k, shape, f32)
    return {
        "x": nrm(ks[0], (BATCH, SEQ, D_MODEL), 1.0),
        "mix_norm_g": gain(ks[1], (DEPTH, D_MODEL)),
        "w_in": nrm(ks[2], (DEPTH, D_MODEL, IN_COLS), D_MODEL ** -0.5),
        "q_norm_g": gain(ks[3], (DEPTH, HEAD_DIM)),
        "k_norm_g": gain(ks[4], (DEPTH, HEAD_DIM)),
        "sgu_v_norm_g": gain(ks[5], (DEPTH, SGU_WIDTH)),
        "sgu_w": nrm(ks[6], (DEPTH, SGU_GROUPS, CHUNK, CHUNK), CHUNK ** -0.5),
        "sgu_b": 1.0 + 0.1 * jax.random.normal(ks[7], (DEPTH, SGU_GROUPS, CHUNK), f32),
        "sb_out_norm_g": gain(ks[8], (DEPTH, SB_WIDTH)),
        "sgu_out_norm_g": gain(ks[9], (DEPTH, SGU_WIDTH)),
        "w_out": nrm(ks[10], (DEPTH, MIX_WIDTH, D_MODEL), MIX_WIDTH ** -0.5),
        "ffn_norm_g": gain(ks[11], (DEPTH, D_MODEL)),
        "peer_w_q": nrm(ks[12], (DEPTH, D_MODEL, PEER_HEADS * PEER_QDIM), D_MODEL ** -0.5),
        "peer_sub_keys": nrm(ks[13], (DEPTH, PEER_HEADS, 2, PEER_NKEYS, PEER_HALF), PEER_HALF ** -0.5),
        "peer_u": nrm(ks[14], (DEPTH, PEER_EXPERTS, D_MODEL), D_MODEL ** -0.5),
        "peer_v": nrm(ks[15], (DEPTH, PEER_EXPERTS, D_MODEL), PEER_HEADS ** -0.5),
    }


def reference(x, mix_norm_g, w_in, q_norm_g, k_norm_g, sgu_v_norm_g, sgu_w, sgu_b,
              sb_out_norm_g, sgu_out_norm_g, w_out, ffn_norm_g, peer_w_q, peer_sub_keys,
              peer_u, peer_v):
    B, S, _ = x.shape
    h = x
    for l in range(DEPTH):
        hn = rms_norm(h, mix_norm_g[l])
        proj = hn @ w_in[l]
        q, k, v, u_s, v_s = jnp.split(
            proj, [SB_WIDTH, 2 * SB_WIDTH, 3 * SB_WIDTH, 3 * SB_WIDTH + SGU_WIDTH], axis=-1)
        to_heads = lambda a: a.reshape(B, S, SB_HEADS, HEAD_DIM).transpose(0, 2, 1, 3)
        qh = rms_norm(to_heads(q), q_norm_g[l])
        kh = rms_norm(to_heads(k), k_norm_g[l])
        sb = stick_breaking_attention(qh, kh, to_heads(v))
        sb = sb.transpose(0, 2, 1, 3).reshape(B, S, SB_WIDTH).astype(x.dtype)
        sgu = spatial_gating(jax.nn.gelu(u_s), jax.nn.gelu(v_s),
                             sgu_v_norm_g[l], sgu_w[l], sgu_b[l])
        mixed = jnp.concatenate([rms_norm(sb, sb_out_norm_g[l]),
                                 rms_norm(sgu, sgu_out_norm_g[l])], axis=-1)
        h = h + mixed @ w_out[l]
        h = h + peer_ffn(rms_norm(h, ffn_norm_g[l]), peer_w_q[l], peer_sub_keys[l],
                         peer_u[l], peer_v[l])
    return h
```

```python
import numpy as np
from contextlib import ExitStack
import concourse.bass as bass
import concourse.mybir as mybir
from concourse.bass_utils import run_bass_kernel_spmd

F32 = mybir.dt.float32
BF16 = mybir.dt.bfloat16
U32 = mybir.dt.uint32
AF = mybir.ActivationFunctionType
ALU = mybir.AluOpType
AX = mybir.AxisListType

ENGS = ("pe", "act", "dve", "pool", "sp")
BLK = {"pe": "tensor", "act": "scalar", "dve": "vector", "pool": "gpsimd", "sp": "sync"}


class _Op:
    __slots__ = ("eng", "fn", "deps", "dma", "inc", "sig", "val", "semname")


class Prog:
    def __init__(self, nc, es):
        self.nc = nc
        self.es = es
        self.ops = []
        self.last_w = {}
        self.readers = {}
        self.eng_cnt = {e: 0 for e in ENGS}
        self.dma_cnt = {}
        self.sems = {}
        self.waited = {e: {} for e in ENGS}
        self.excl = set()

    def sem(self, name):
        if name not in self.sems:
            self.sems[name] = self.es.enter_context(self.nc.semaphore(name))
        return self.sems[name]

    def add(self, eng, fn, r=(), w=(), dma=None, inc=16):
        i = len(self.ops)
        deps = {}
        w = list(w) + [k for k in r if k in self.excl]
        r = [k for k in r if k not in self.excl]
        for k in r:
            if k in self.last_w:
                deps[self.last_w[k]] = True
        for k in w:
            if k in self.last_w:
                deps[self.last_w[k]] = True
            for d in self.readers.get(k, {}).values():
                if d not in deps:
                    deps[d] = False
        op = _Op()
        op.eng, op.fn, op.deps, op.dma, op.inc = eng, fn, deps, dma, inc
        op.sig, op.val, op.semname = False, 0, None
        self.ops.append(op)
        rk = ("dma", i) if dma is not None else eng
        for k in r:
            self.readers.setdefault(k, {})[rk] = i
        for k in w:
            self.last_w[k] = i
            self.readers[k] = {}
        return i

    def dma(self, eng, out, in_, r, w, sem, **kw):
        return self.add(eng, lambda e: e.dma_start(out=out, in_=in_, **kw), r, w, dma=sem)

    def emit(self):
        ops = self.ops
        for op in ops:
            for d in op.deps:
                if ops[d].dma is None:
                    ops[d].sig = True
        last = {}
        for i, op in enumerate(ops):
            if op.dma is None:
                last[op.eng] = i
        for i in last.values():
            ops[i].sig = True
        final = {}
        for op in ops:
            if op.dma is not None:
                op.semname = "d_" + op.dma
                self.dma_cnt[op.dma] = self.dma_cnt.get(op.dma, 0) + op.inc
                op.val = self.dma_cnt[op.dma]
                final[op.semname] = op.val
            elif op.sig:
                op.semname = "e_" + op.eng
                self.eng_cnt[op.eng] += 1
                op.val = self.eng_cnt[op.eng]
                final[op.semname] = op.val
        for s in final:
            self.sem(s)
        with self.nc.Block() as block:
            for eng in ENGS:
                my = [op for op in ops if op.eng == eng]

                def body(e, my=my, eng=eng):
                    for op in my:
                        for d in sorted(op.deps):
                            p = ops[d]
                            if p.dma is None and p.eng == eng and (eng == "pe" or not op.deps[d]):
                                continue
                            self._wait(e, eng, p.semname, p.val)
                        ins = op.fn(e)
                        if op.dma is not None:
                            ins.then_inc(self.sems[op.semname], op.inc)
                        elif op.sig:
                            ins.then_inc(self.sems[op.semname], 1)
                    for s, v in final.items():
                        self._wait(e, eng, s, v)

                getattr(block, BLK[eng])(body)
        self.ops = []
        self.last_w = {}
        self.readers = {}

    def _wait(self, e, eng, semname, val):
        if self.waited[eng].get(semname, 0) >= val:
            return
        e.wait_ge(self.sems[semname], val)
        self.waited[eng][semname] = val


D = 4096
NT = 8
TOK = 1024
NH = 16
EPS = 1e-6
NCORES = 8
QSCALE = 128 ** -0.5


def bc_rows(handle, off, n, parts=128):
    return bass.AP(handle, off, [[0, parts], [1, n]])


def build(stop_after=None, dbg=False, with_peer=True, only_B=False, nheads=NH, only_DE=False):
    nc = bass.Bass("TRN2", target_bir_lowering=False)
    dt = nc.dram_tensor
    x = dt("x", [TOK, D], F32, kind="ExternalInput")
    w_in_sh = dt("w_in", [D // NCORES, 10240], F32, kind="ExternalInput")
    w_in_b = dt("w_in_b", [D // NCORES, 10240], BF16)
    w_in = dt("w_in_g", [D, 10240], BF16)
    mix_g = dt("mix_norm_g", [1, D], F32, kind="ExternalInput")
    q_g = dt("q_norm_g", [1, 128], F32, kind="ExternalInput")
    k_g = dt("k_norm_g", [1, 128], F32, kind="ExternalInput")
    vn_g = dt("sgu_v_norm_g", [1, 2048], F32, kind="ExternalInput")
    sgu_w = dt("sgu_w", [16, 128, 128], F32, kind="ExternalInput")
    sgu_b = dt("sgu_b", [16, 128], F32, kind="ExternalInput")
    sbo_g = dt("sb_out_norm_g", [1, 2048], F32, kind="ExternalInput")
    sgo_g = dt("sgu_out_norm_g", [1, 2048], F32, kind="ExternalInput")
    w_out_sh = dt("w_out", [D // NCORES, D], F32, kind="ExternalInput")
    w_out_b = dt("w_out_b", [D // NCORES, D], BF16)
    w_out = dt("w_out_g", [D, D], BF16)
    ffn_g = dt("ffn_norm_g", [1, D], F32, kind="ExternalInput")
    w_q_sh = dt("peer_w_q", [D // NCORES, D], F32, kind="ExternalInput")
    w_q_b = dt("w_q_b", [D // NCORES, D], BF16)
    w_q = dt("w_q_g", [D, D], BF16)
    subk = dt("peer_sub_keys", [8, 2, 128, 256], F32, kind="ExternalInput")
    if with_peer:
        pu_sh = dt("peer_u", [16384 // NCORES, D], F32, kind="ExternalInput")
        pv_sh = dt("peer_v", [16384 // NCORES, D], F32, kind="ExternalInput")
        pu_b = dt("pu_b", [16384 // NCORES, D], BF16)
        pv_b = dt("pv_b", [16384 // NCORES, D], BF16)
        pu = dt("pu_g", [16384, D], BF16)
        pv = dt("pv_g", [16384, D], BF16)
    ident_d = dt("ident", [128, 128], F32, kind="ExternalInput")
    amask_d = dt("amask", [128, 8, 128], F32, kind="ExternalInput")
    y = dt("y", [TOK, D], F32, kind="ExternalOutput")

    kT_loc = dt("kT_loc", [NH * 128, TOK], BF16)
    v_loc = dt("v_loc", [NH * NT * 128, 128], BF16)
    kw_b = dict(kind="ExternalInput") if only_B else {}
    kT_all = dt("kT_all", [NCORES * NH * 128, TOK], BF16, **kw_b)
    v_all = dt("v_all", [NCORES * NH * NT * 128, 128], BF16, **kw_b)
    qT_scr = dt("qT_scr", [NH * 128, TOK], BF16, **kw_b)
    sgu_scr = dt("sgu_scr", [TOK, 2048], BF16)
    h_scr = dt("h_scr", [TOK, D], F32, **(dict(kind="ExternalInput") if only_DE else {}))
    G_scr = dt("G_scr", [128 * 128, TOK], BF16)
    coef_scr = dt("coef_scr", [128 * 128, TOK], BF16)
    dbg_t = {}
    if dbg:
        dbg_t["hnT"] = dt("dbg_hnT", [128, 32 * TOK], BF16, kind="ExternalOutput")
        dbg_t["qT"] = dt("dbg_qT", [NH * 128, TOK], BF16, kind="ExternalOutput")
        dbg_t["kT_all"] = dt("dbg_kT_all", [NCORES * NH * 128, TOK], BF16, kind="ExternalOutput")
        dbg_t["v_all"] = dt("dbg_v_all", [NCORES * NH * NT * 128, 128], BF16, kind="ExternalOutput")
        dbg_t["sgu"] = dt("dbg_sgu", [TOK, 2048], BF16, kind="ExternalOutput")
        dbg_t["ssg"] = dt("dbg_ssg", [128, 32], F32, kind="ExternalOutput")
        dbg_t["sbgT"] = dt("dbg_sbgT", [128, NH * TOK], BF16, kind="ExternalOutput")
        dbg_t["ss_sb"] = dt("dbg_ss_sb", [128, NT], F32, kind="ExternalOutput")
        dbg_t["h"] = dt("dbg_h", [TOK, D], F32, kind="ExternalOutput")
        dbg_t["h2T"] = dt("dbg_h2T", [128, 32 * TOK], BF16, kind="ExternalOutput")
        dbg_t["gs"] = dt("dbg_gs", [128, 5 * 1024], F32, kind="ExternalOutput")
        if with_peer:
            dbg_t["G"] = dt("dbg_G", [128 * 128, TOK], BF16, kind="ExternalOutput")

    es0 = ExitStack()
    P = Prog(nc, es0)
    sb = lambda es, name, shape, dtype: es.enter_context(nc.sbuf_tensor(name, shape, dtype))
    def ps(es, name, shape, dtype):
        full = [128, 512] if dtype == F32 else [128, 1024]
        P.excl.add(name)
        return es.enter_context(nc.psum_tensor(name, full, dtype))

    ident_f = sb(es0, "ident_f", [128, 128], F32)
    ident = sb(es0, "ident_b", [128, 128], BF16)
    ssg_part = sb(es0, "ssg_part", [128, NT, 4], F32)
    rstd_x = sb(es0, "rstd_x", [128, NT], F32)
    ss_sb = sb(es0, "ss_sb", [128, NT], F32)
    ssh_part = sb(es0, "ssh_part", [128, NT, 8], F32)

    def cast_shard(dst, src_, rows, key):
        step = 256
        for r0 in range(0, rows, step):
            P.dma("pool", dst[r0:r0 + step, :], src_[r0:r0 + step, :], r=[], w=[key], sem=key, max_dma_last_dim=8192)

    def rstd_ops(t1, t2, ss_ap, dim, key_in, key_out, out_ap, tag):
        P.add("dve", lambda e: e.tensor_scalar(t1, ss_ap, 1.0 / dim, EPS, ALU.mult, ALU.add),
              r=[key_in], w=[tag + "1"])
        P.add("act", lambda e: e.activation(out=t2, in_=t1, func=AF.Sqrt), r=[tag + "1"], w=[tag + "2"])
        P.add("dve", lambda e: e.reciprocal(out_ap, t2), r=[tag + "2"], w=[key_out])

    if not only_B and not only_DE:
        with ExitStack() as es:
            gq = sb(es, "gq", [128, 128], F32)
            gk = sb(es, "gk", [128, 128], F32)
            gvn = sb(es, "gvn", [128, 2048], F32)
            bsg = sb(es, "bsg", [128, 16], F32)
            wcT = sb(es, "wcT", [128, 16, 128], BF16)
            hnT = sb(es, "hnT", [128, 32, TOK], BF16)
            ss_x = sb(es, "ss_x", [128, NT], F32)
            t1 = sb(es, "t1", [128, NT], F32)
            t2 = sb(es, "t2", [128, NT], F32)
            sq = sb(es, "sq", [128, 512], F32)
            ss4 = sb(es, "ss4", [128, 4], F32)
            r4 = sb(es, "r4", [128, 4], F32)
            t41 = sb(es, "t41", [128, 4], F32)
            t42 = sb(es, "t42", [128, 4], F32)
            qn = sb(es, "qn", [128, 4, 128], BF16)
            vb = sb(es, "vb", [128, 512], BF16)
            gv = sb(es, "gv", [128, 512], F32)
            vnb = sb(es, "vnb", [128, 4, 128], BF16)
            sgu_f = sb(es, "sgu_f", [128, 512], F32)
            sgu_bf = sb(es, "sgu_bf", [128, 512], BF16)
            tp = [ps(es, f"tp{i}", [128, 512], BF16) for i in range(2)]
            mm = [ps(es, f"mm{i}", [128, 512], F32) for i in range(3)]
            ps2 = ps(es, "ps2", [128, 512], F32)

            es1 = ExitStack()
            gmix = sb(es1, "gmix", [128, D], F32)
            wc_f = sb(es1, "wc_f", [128, 16, 128], F32)
            wc_b = sb(es1, "wc_b", [128, 16, 128], BF16)
            xt = sb(es1, "xt", [128, D], F32)
            hn = sb(es1, "hn", [128, D], BF16)
            cast_shard(w_in_b, w_in_sh, D // NCORES, "w_in_b")
            P.add("pool", lambda e: e.collective_compute("AllGather", ALU.bypass, replica_groups=[list(range(NCORES))],
                                                         ins=[w_in_b.ap().opt()], outs=[w_in.ap().opt()]),
                  r=["w_in_b"], w=["w_in_g"], dma="ccw0", inc=1)
            P.dma("sp", ident_f[:], ident_d[:, :], r=[], w=["ident_f"], sem="c0")
            P.add("dve", lambda e: e.tensor_copy(ident[:], ident_f[:]), r=["ident_f"], w=["ident"])
            P.dma("sp", gmix[:], bc_rows(mix_g, 0, D), r=[], w=["gmix"], sem="c1")
            P.dma("sp", gq[:], bc_rows(q_g, 0, 128), r=[], w=["gq"], sem="c2")
            P.add("dve", lambda e: e.tensor_scalar(gq[:], gq[:], QSCALE, None, ALU.mult), r=["gq"], w=["gq"])
            P.dma("sp", gk[:], bc_rows(k_g, 0, 128), r=[], w=["gk"], sem="c3")
            P.dma("sp", gvn[:], bc_rows(vn_g, 0, 2048), r=[], w=["gvn"], sem="c4")
            P.add("sp", lambda e: e.dma_start(out=bsg[:], in_=sgu_b.ap().rearrange("g t -> t g"),
                                              allow_slow_non_contiguous=True), r=[], w=["bsg"], dma="c5")
            P.dma("sp", wc_f[:], sgu_w.ap().rearrange("g t s -> t g s"), r=[], w=["wc_f"], sem="c6")
            P.add("pool", lambda e: e.affine_select(out=wc_f[:], in_=wc_f[:], pattern=[[0, 16], [-1, 128]],
                                                    compare_op=ALU.is_ge, fill=0.0, base=0, channel_multiplier=1),
                  r=["wc_f"], w=["wc_f"])
            P.add("dve", lambda e: e.tensor_copy(wc_b[:], wc_f[:]), r=["wc_f"], w=["wc_b"])
            for g4 in range(4):
                for i in range(4):
                    g = g4 * 4 + i
                    P.add("pe", lambda e, g=g, i=i, g4=g4: e.transpose(tp[g4 % 2][:, i * 128:(i + 1) * 128], wc_b[:, g, :], ident[:]),
                          r=["wc_b", "ident"], w=[f"tp{g4 % 2}"])
                P.add("dve", lambda e, g4=g4: e.tensor_copy(wcT[:, g4 * 4:(g4 + 1) * 4, :],
                                                           tp[g4 % 2][:, 0:512].rearrange("p (a b) -> p a b", a=4)),
                      r=[f"tp{g4 % 2}"], w=["wcT"])

            for j in range(NT):
                P.dma("sp", xt[:], x[j * 128:(j + 1) * 128, :], r=[], w=["xt"], sem="xt")
                P.add("act", lambda e, j=j: e.activation(out=hn[:], in_=xt[:], func=AF.Square, accum_out=ss_x[:, j:j + 1]),
                      r=["xt"], w=["hn", f"ss_x{j}"])
                rstd_ops(t1[:, j:j + 1], t2[:, j:j + 1], ss_x[:, j:j + 1], D, f"ss_x{j}", f"rstd_x{j}", rstd_x[:, j:j + 1], f"rx{j}")
                P.add("dve", lambda e, j=j: e.scalar_tensor_tensor(out=hn[:], in0=xt[:], scalar=rstd_x[:, j:j + 1], in1=gmix[:],
                                                                  op0=ALU.mult, op1=ALU.mult),
                      r=["xt", f"rstd_x{j}", "gmix", "hn"], w=["hn"])
                for g in range(8):
                    b = g % 2
                    for i in range(4):
                        dc = g * 4 + i
                        P.add("pe", lambda e, dc=dc, i=i, b=b: e.transpose(tp[b][:, i * 128:(i + 1) * 128], hn[:, dc * 128:(dc + 1) * 128], ident[:]),
                              r=["hn", "ident"], w=[f"tp{b}"])
                    eng = "act" if g % 2 == 0 else "dve"
                    if eng == "act":
                        P.add("act", lambda e, g=g, j=j, b=b: e.copy(out=hnT[:, g * 4:(g + 1) * 4, j * 128:(j + 1) * 128],
                                                                    in_=tp[b][:, 0:512].rearrange("p (a b) -> p a b", a=4)),
                              r=[f"tp{b}"], w=["hnT"])
                    else:
                        P.add("dve", lambda e, g=g, j=j, b=b: e.tensor_copy(hnT[:, g * 4:(g + 1) * 4, j * 128:(j + 1) * 128],
                                                                           tp[b][:, 0:512].rearrange("p (a b) -> p a b", a=4)),
                              r=[f"tp{b}"], w=["hnT"])
            if dbg:
                P.dma("sp", dbg_t["hnT"][:, :], hnT[:].rearrange("p a b -> p (a b)"), r=["hnT"], w=["dbg_hnT"], sem="dbg")

            P.emit()
            es1.close()
            wbuf = [sb(es, f"wbuf{i}", [128, 32, 512], BF16) for i in range(2)]
            qTc = sb(es, "qTc", [128, 4, TOK], BF16)
            u_sb = sb(es, "u_sb", [128, NT, 512], BF16)
            chunks = [("q", i, 512 * i) for i in range(4)] + [("k", i, 2048 + 512 * i) for i in range(4)] + \
                     [("v", i, 4096 + 512 * i) for i in range(4)]
            for i in range(4):
                chunks += [("u", i, 6144 + 512 * i), ("s", i, 8192 + 512 * i)]
            w_view = w_in.ap().rearrange("(dc p) n -> p dc n", p=128)

            def load_w(ci):
                kind, i, col0 = chunks[ci]
                b = ci % 2
                P.dma("pool", wbuf[b][:], w_view[:, :, col0:col0 + 512], r=["w_in_g"], w=[f"wbuf{b}"], sem=f"wbuf{b}")

            load_w(0)
            mmi = 0
            for ci, (kind, i, col0) in enumerate(chunks):
                if ci + 1 < len(chunks):
                    load_w(ci + 1)
                b = ci % 2
                for j in range(NT):
                    pm = mm[mmi % 3]
                    pk = f"mm{mmi % 3}"
                    mmi += 1
                    for dc in range(32):
                        P.add("pe", lambda e, dc=dc, j=j, b=b, pm=pm: e.matmul(pm[:], hnT[:, dc, j * 128:(j + 1) * 128], wbuf[b][:, dc, :],
                                                                            start=(dc == 0), stop=(dc == 31)),
                              r=["hnT", f"wbuf{b}"], w=[pk])
                    if kind in ("q", "k"):
                        gg = gq if kind == "q" else gk
                        P.add("act", lambda e, pm=pm: e.activation(out=sq[:], in_=pm[:], func=AF.Square), r=[pk], w=["sq"])
                        P.add("dve", lambda e: e.reduce_sum(out=ss4[:], in_=sq[:].rearrange("p (a b) -> p a b", a=4), axis=AX.X),
                              r=["sq"], w=["ss4"])
                        rstd_ops(t41[:], t42[:], ss4[:], 128, "ss4", "r4", r4[:], "r4t")
                        for hh in range(4):
                            P.add("dve", lambda e, hh=hh, pm=pm, gg=gg: e.scalar_tensor_tensor(
                                out=qn[:, hh, :], in0=pm[:, hh * 128:(hh + 1) * 128], scalar=r4[:, hh:hh + 1], in1=gg[:],
                                op0=ALU.mult, op1=ALU.mult), r=[pk, "r4", "gq", "gk"], w=["qn"])
                        tb = j % 2
                        for hh in range(4):
                            P.add("pe", lambda e, hh=hh, tb=tb: e.transpose(tp[tb][:, hh * 128:(hh + 1) * 128], qn[:, hh, :], ident[:]),
                                  r=["qn", "ident"], w=[f"tp{tb}"])
                        P.add("act", lambda e, j=j, tb=tb: e.copy(out=qTc[:, :, j * 128:(j + 1) * 128],
                                                                 in_=tp[tb][:, 0:512].rearrange("p (a b) -> p a b", a=4)),
                              r=[f"tp{tb}"], w=["qTc"])
                    elif kind == "v":
                        P.add("act", lambda e, pm=pm: e.copy(out=vb[:], in_=pm[:]), r=[pk], w=["vb"])
                        dst = v_loc.ap().rearrange("(h jb s) d -> s h jb d", h=NH, jb=NT)[:, 4 * i:4 * i + 4, j, :]
                        P.dma("sp", dst, vb[:].rearrange("p (h d) -> p h d", h=4), r=["vb"], w=["v_loc"], sem="v_loc")
                    elif kind == "u":
                        P.add("act", lambda e, pm=pm, j=j: e.activation(out=u_sb[:, j, :], in_=pm[:], func=AF.Gelu_apprx_tanh),
                              r=[pk], w=["u_sb"])
                    else:
                        P.add("act", lambda e, pm=pm: e.activation(out=gv[:], in_=pm[:], func=AF.Gelu_apprx_tanh), r=[pk], w=["gv"])
                        P.add("act", lambda e: e.activation(out=sq[:], in_=gv[:], func=AF.Square), r=["gv"], w=["sq"])
                        P.add("dve", lambda e: e.reduce_sum(out=ss4[:], in_=sq[:].rearrange("p (a b) -> p a b", a=4), axis=AX.X),
                              r=["sq"], w=["ss4"])
                        rstd_ops(t41[:], t42[:], ss4[:], 128, "ss4", "r4", r4[:], "r4t")
                        for g4 in range(4):
                            g = 4 * i + g4
                            P.add("dve", lambda e, g4=g4, g=g: e.scalar_tensor_tensor(
                                out=vnb[:, g4, :], in0=gv[:, g4 * 128:(g4 + 1) * 128], scalar=r4[:, g4:g4 + 1],
                                in1=gvn[:, g * 128:(g + 1) * 128], op0=ALU.mult, op1=ALU.mult), r=["gv", "r4", "gvn"], w=["vnb"])
                        for g4 in range(4):
                            g = 4 * i + g4
                            P.add("pe", lambda e, g4=g4, g=g: e.matmul(ps2[:, g4 * 128:(g4 + 1) * 128], wcT[:, g, :], vnb[:, g4, :],
                                                                      start=True, stop=True), r=["wcT", "vnb"], w=["ps2"])
                        for g4 in range(4):
                            g = 4 * i + g4
                            P.add("dve", lambda e, g4=g4, g=g, j=j: e.scalar_tensor_tensor(
                                out=sgu_f[:, g4 * 128:(g4 + 1) * 128], in0=ps2[:, g4 * 128:(g4 + 1) * 128], scalar=bsg[:, g:g + 1],
                                in1=u_sb[:, j, g4 * 128:(g4 + 1) * 128], op0=ALU.add, op1=ALU.mult),
                                r=["ps2", "bsg", "u_sb"], w=["sgu_f"])
                        P.add("act", lambda e, j=j, i=i: e.activation(out=sq[:], in_=sgu_f[:], func=AF.Square,
                                                                     accum_out=ssg_part[:, j, i:i + 1]),
                              r=["sgu_f"], w=["sq", "ssg_part"])
                        P.add("dve", lambda e: e.tensor_copy(sgu_bf[:], sgu_f[:]), r=["sgu_f"], w=["sgu_bf"])
                        P.dma("sp", sgu_scr[j * 128:(j + 1) * 128, 512 * i:512 * (i + 1)], sgu_bf[:], r=["sgu_bf"], w=["sgu_scr"],
                              sem="sgu_scr")
                if kind in ("q", "k"):
                    dst_t = qT_scr if kind == "q" else kT_loc
                    dst = dst_t.ap().rearrange("(h d) t -> d h t", d=128)[:, 4 * i:4 * i + 4, :]
                    P.dma("sp", dst, qTc[:], r=["qTc"], w=[dst_t.name], sem=dst_t.name)
            P.emit()

        P.add("pool", lambda e: e.collective_compute("AllGather", ALU.bypass, replica_groups=[list(range(NCORES))],
                                                     ins=[kT_loc.ap().opt()], outs=[kT_all.ap().opt()]),
              r=[], w=["kT_all"], dma="cc1", inc=1)
        P.add("pool", lambda e: e.collective_compute("AllGather", ALU.bypass, replica_groups=[list(range(NCORES))],
                                                     ins=[v_loc.ap().opt()], outs=[v_all.ap().opt()]),
              r=[], w=["v_all"], dma="cc2", inc=1)
        if dbg:
            P.dma("sp", dbg_t["kT_all"][:, :], kT_all[:, :], r=["kT_all"], w=["d1"], sem="dbg")
            P.dma("sp", dbg_t["v_all"][:, :], v_all[:, :], r=["v_all"], w=["d2"], sem="dbg")
            P.dma("sp", dbg_t["qT"][:, :], qT_scr[:, :], r=[], w=["d3"], sem="dbg")
            P.dma("sp", dbg_t["sgu"][:, :], sgu_scr[:, :], r=[], w=["d4"], sem="dbg")
            P.dma("sp", dbg_t["ssg"][:, :], ssg_part[:].rearrange("p a b -> p (a b)"), r=[], w=["d5"], sem="dbg")
        P.emit()
    def finish():
        with ExitStack() as es:
            xo = sb(es, "xt_o", [128, D], F32)
            P.dma("sp", xo[:], x[0:128, :], r=[], w=["xo"], sem="xo")
            P.dma("sp", y[0:128, :], xo[:], r=["xo"], w=["y"], sem="y")
            P.emit()
        return nc

    if stop_after == "A":
        return finish()

    if not only_DE:
        esBC = ExitStack()
        sbgT = sb(esBC, "sbgT", [128, NH, TOK], BF16)
        with ExitStack() as es:
            KT = [sb(es, f"KT{i}", [128, 8, TOK], BF16) for i in range(2)]
            VV = [sb(es, f"VV{i}", [128, 8, NT, 128], BF16) for i in range(2)]
            QT = [sb(es, f"QT{i}", [128, TOK], BF16) for i in range(2)]
            am_f = sb(es, "am_f", [128, 8, 128], F32)
            am = sb(es, "am", [128, 8, 128], BF16)
            tri_f = sb(es, "tri_f", [128, 128], F32)
            negtri = sb(es, "negtri", [128, 128], BF16)
            negones = sb(es, "negones", [128, 128], BF16)
            ones_col = sb(es, "ones_col", [128, 1], BF16)
            one_c = sb(es, "one_c", [128, 1], F32)
            gsbo = sb(es, "gsbo", [128, NH], F32)
            Eb = [sb(es, f"Eb{i}", [128, 512], F32) for i in range(2)]
            Lp = [sb(es, f"Lp{i}", [128, 512], BF16) for i in range(2)]
            Ab = [sb(es, f"Ab{i}", [128, 512], BF16) for i in range(2)]
            Lsum = sb(es, "Lsum", [128, 512], BF16)
            sqT = sb(es, "sqT", [128, 512], BF16)
            zA = [ps(es, f"zA{i}", [128, 512], F32) for i in range(2)]
            zB = [ps(es, f"zB{i}", [128, 512], F32) for i in range(2)]
            oT = ps(es, "oT", [128, 512], F32)
            ssps = ps(es, "ssps", [128, NT], F32)

            import os
            SKIP = os.environ.get("SKIP", "").split(",")
            if not only_B:
                gl = [(w_out_b, w_out, "ccw1"), (w_q_b, w_q, "ccw2")]
                if with_peer:
                    gl += [(pu_b, pu, "ccw3"), (pv_b, pv, "ccw4")]
                for (s_, d_, key_) in gl:
                    sh_ = {"w_out_b": w_out_sh, "w_q_b": w_q_sh}.get(s_.name)
                    if sh_ is None:
                        sh_ = pu_sh if s_.name == "pu_b" else pv_sh
                    cast_shard(s_, sh_, s_.shape[0], s_.name)
                    P.add("pool", lambda e, s_=s_, d_=d_: e.collective_compute("AllGather", ALU.bypass, replica_groups=[list(range(NCORES))],
                                                                             ins=[s_.ap().opt()], outs=[d_.ap().opt()]),
                          r=[s_.name], w=[d_.name], dma=key_, inc=1)
            P.dma("sp", am_f[:], amask_d[:, :, :], r=[], w=["am_f"], sem="c0")
            P.add("dve", lambda e: e.tensor_copy(am[:], am_f[:]), r=["am_f"], w=["am"])
            P.add("pool", lambda e: e.memset(tri_f[:], -1.0), r=[], w=["tri_f"])
            if "tri" not in SKIP:
                P.add("pool", lambda e: e.affine_select(out=tri_f[:], in_=tri_f[:], pattern=[[-1, 128]], compare_op=ALU.is_ge,
                                                        fill=0.0, base=0, channel_multiplier=1), r=["tri_f"], w=["tri_f"])
            P.add("dve", lambda e: e.tensor_copy(negtri[:], tri_f[:]), r=["tri_f"], w=["negtri"])
            P.add("pool", lambda e: e.memset(negones[:], -1.0), r=[], w=["negones"])
            P.add("pool", lambda e: e.memset(ones_col[:], 1.0), r=[], w=["ones_col"])
            P.add("pool", lambda e: e.memset(one_c[:], 1.0), r=[], w=["one_c"])
            if "gsbo" not in SKIP:
                P.add("sp", lambda e: e.dma_start(out=gsbo[:], in_=sbo_g.ap().rearrange("o (h d) -> d (o h)", d=128),
                                                  allow_slow_non_contiguous=True), r=[], w=["gsbo"], dma="c1")

            P.add("dve", lambda e: e.memset(ssps[:], 0.0), r=[], w=["ssps"])
            kT_v = kT_all.ap().rearrange("(r h d) t -> d r h t", r=NCORES, h=NH)
            v_v = v_all.ap().rearrange("(r h jb s) d -> s r h jb d", r=NCORES, h=NH, jb=NT)
            qT_v = qT_scr.ap().rearrange("(h d) t -> d h t", d=128)

            def load_head(h):
                hb = h % 2
                P.dma("sp", KT[hb][:], kT_v[:, :, h, :], r=["kT_all"], w=[f"KT{hb}"], sem=f"KT{hb}")
                for r_ in range(NCORES):
                    P.dma("sp", VV[hb][:, r_, :, :], v_v[:, r_, h, :, :], r=["v_all"], w=[f"VV{hb}"], sem=f"VV{hb}")
                P.dma("sp", QT[hb][:], qT_v[:, h, :], r=[], w=[f"QT{hb}"], sem=f"QT{hb}")

            if "lh" not in SKIP:
                load_head(0)
            u = 0
            for h in range(nheads):
                if h + 1 < nheads:
                    load_head(h + 1)
                hb = h % 2
                for qb in range(2):
                    c0 = 512 * qb
                    j0 = 4 * qb
                    P.add("pool", lambda e: e.memset(Lsum[:], 0.0), r=[], w=["Lsum"])
                    P.add("dve", lambda e: e.memset(oT[:], 0.0), r=[], w=["oT"])
                    for kb in range(8 * (j0 + 3) + 7, -1, -1):
                        jmin = max(j0, (kb - 7 + 7) // 8)
                        a0 = 128 * (jmin - j0)
                        jstar, m = kb // 8, kb % 8
                        ms = 128 * (jstar - j0) if jstar >= j0 else None
                        r_, jb = kb % 8, kb // 8
                        pb = u % 2
                        if u >= int(os.environ.get("MAXU", "100000")):
                            continue
                        u += 1
                        UCUT = int(os.environ.get("UCUT", "100"))
                        kblk = KT[hb][:, r_, jb * 128:(jb + 1) * 128]
                        qcols = QT[hb][:, c0 + a0:c0 + 512]
                        rk = [f"KT{hb}", f"QT{hb}"]
                        P.add("pe", lambda e, pb=pb, a0=a0, kblk=kblk, qcols=qcols: e.matmul(zA[pb][:, a0:512], kblk, qcols, start=True, stop=True),
                              r=rk, w=[f"zA{pb}"])
                        if UCUT <= 1:
                            continue
                        P.add("act", lambda e, pb=pb, a0=a0: e.activation(out=Eb[pb][:, a0:512], in_=zA[pb][:, a0:512], func=AF.Exp),
                              r=[f"zA{pb}"], w=[f"Eb{pb}"])
                        if UCUT <= 2:
                            continue
                        P.add("act", lambda e, pb=pb, a0=a0: e.activation(out=Lp[pb][:, a0:512], in_=Eb[pb][:, a0:512], func=AF.Ln,
                                                                         bias=one_c[:, 0:1], scale=1.0),
                              r=[f"Eb{pb}", "one_c"], w=[f"Lp{pb}"])
                        if UCUT <= 3:
                            continue
                        if ms is not None:
                            if os.environ.get("MVAR", "") == "oop":
                                P.add("dve", lambda e, pb=pb, ms=ms, m=m: e.tensor_tensor(out=Ab[pb][:, ms:ms + 128], in0=Lp[pb][:, ms:ms + 128],
                                                                                     in1=am[:, m, :], op=ALU.mult),
                                  r=[f"Lp{pb}", "am"], w=[f"Ab{pb}"])
                            elif os.environ.get("MVAR", "") == "cp":
                                P.add("dve", lambda e, pb=pb, ms=ms, m=m: e.tensor_copy(Ab[pb][:, ms:ms + 128], Lp[pb][:, ms:ms + 128]),
                                  r=[f"Lp{pb}"], w=[f"Ab{pb}"])
                            elif os.environ.get("MVAR", "") == "amonly":
                                P.add("dve", lambda e, pb=pb, ms=ms, m=m: e.tensor_copy(Ab[pb][:, ms:ms + 128], am[:, m, :]),
                                  r=["am"], w=[f"Ab{pb}"])
                            elif os.environ.get("MVAR", "") == "amf":
                                P.add("dve", lambda e, pb=pb, ms=ms, m=m: e.tensor_tensor(out=Lp[pb][:, ms:ms + 128], in0=Lp[pb][:, ms:ms + 128],
                                                                                     in1=am_f[:, m, :], op=ALU.mult),
                                  r=[f"Lp{pb}", "am"], w=[f"Lp{pb}"])
                            else:
                                P.add(os.environ.get("MENG", "dve"), lambda e, pb=pb, ms=ms, m=m: e.tensor_tensor(out=Lp[pb][:, ms:ms + 128], in0=Lp[pb][:, ms:ms + 128],
                                                                                     in1=am[:, m, :], op=ALU.mult),
                                  r=[f"Lp{pb}", "am"], w=[f"Lp{pb}"])
                        if UCUT <= 4:
                            continue
                        P.add("pe", lambda e, pb=pb, a0=a0, kblk=kblk, qcols=qcols: e.matmul(zB[pb][:, a0:512], kblk, qcols, start=True, stop=False),
                              r=rk, w=[f"zB{pb}"])
                        P.add("pe", lambda e, pb=pb, a0=a0: e.matmul(zB[pb][:, a0:512], negtri[:], Lp[pb][:, a0:512], start=False, stop=False),
                              r=["negtri", f"Lp{pb}"], w=[f"zB{pb}"])
                        P.add("pe", lambda e, pb=pb, a0=a0: e.matmul(zB[pb][:, a0:512], negones[:], Lsum[:, a0:512], start=False, stop=True),
                              r=["negones", "Lsum"], w=[f"zB{pb}"])
                        if UCUT <= 5:
                            continue
                        P.add("act", lambda e, pb=pb, a0=a0: e.activation(out=Ab[pb][:, a0:512], in_=zB[pb][:, a0:512], func=AF.Exp),
                              r=[f"zB{pb}"], w=[f"Ab{pb}"])
                        if UCUT <= 6:
                            continue
                        if ms is not None:
                            P.add("dve", lambda e, pb=pb, ms=ms, m=m: e.tensor_tensor(out=Ab[pb][:, ms:ms + 128], in0=Ab[pb][:, ms:ms + 128],
                                                                                     in1=am[:, m, :], op=ALU.mult),
                                  r=[f"Ab{pb}", "am"], w=[f"Ab{pb}"])
                        if UCUT <= 7:
                            continue
                        vblk = VV[hb][:, r_, jb, :]
                        last = (kb == 0)
                        P.add("pe", lambda e, pb=pb, a0=a0, vblk=vblk: e.matmul(oT[:, a0:512], vblk, Ab[pb][:, a0:512],
                                                                              start=False, stop=False, skip_group_check=True),
                              r=[f"VV{hb}", f"Ab{pb}"], w=["oT"])
                        if UCUT <= 8:
                            continue
                        P.add("pool", lambda e, pb=pb, a0=a0: e.tensor_tensor(out=Lsum[:, a0:512], in0=Lsum[:, a0:512], in1=Lp[pb][:, a0:512], op=ALU.add),
                              r=["Lsum", f"Lp{pb}"], w=["Lsum"])
                    if os.environ.get("NOEPI"):
                        continue
                    P.add("dve", lambda e, h=h, c0=c0: e.tensor_scalar(sbgT[:, h, c0:c0 + 512], oT[:], gsbo[:, h:h + 1], None, ALU.mult),
                          r=["oT", "gsbo"], w=["sbgT"])
                    P.add("act", lambda e: e.activation(out=sqT[:], in_=oT[:], func=AF.Square), r=["oT"], w=["sqT"])
                    for jj in range(4):
                        j = j0 + jj
                        P.add("pe", lambda e, jj=jj, j=j, h=h: e.matmul(ssps[:, j:j + 1], sqT[:, jj * 128:(jj + 1) * 128], ones_col[:, 0:1],
                                                                       start=False, stop=False, skip_group_check=True),
                              r=["sqT", "ones_col"], w=["ssps"])
            if "ssc" not in SKIP:
                P.add("dve", lambda e: e.tensor_copy(ss_sb[:], ssps[:, 0:NT]), r=["ssps"], w=["ss_sb"])
            if dbg:
                P.dma("sp", dbg_t["sbgT"][:, :], sbgT[:].rearrange("p a b -> p (a b)"), r=["sbgT"], w=["d6"], sem="dbg")
                P.dma("sp", dbg_t["ss_sb"][:, :], ss_sb[:], r=["ss_sb"], w=["d7"], sem="dbg")
            P.emit()
        if stop_after == "B":
            esBC.close()
            return finish()

        with ExitStack() as es:
            sggT = sb(es, "sggT", [128, 16, TOK], BF16)
            wbuf = [sb(es, f"wbufc{i}", [128, 32, 512], BF16) for i in range(2)]
            gsgo = sb(es, "gsgo", [128, 2048], F32)
            sg_t = sb(es, "sg_t", [128, 2048], BF16)
            sg_g = sb(es, "sg_g", [128, 2048], BF16)
            ssg = sb(es, "ssg", [128, NT], F32)
            rstd_sb = sb(es, "rstd_sb", [128, NT], F32)
            rstd_sg = sb(es, "rstd_sg", [128, NT], F32)
            c1 = sb(es, "c1t1", [128, NT], F32)
            c2 = sb(es, "c1t2", [128, NT], F32)
            xch = [sb(es, f"xch{i}", [128, 512], F32) for i in range(2)]
            hA = sb(es, "hA", [128, 512], F32)
            hch = [sb(es, f"hch{i}", [128, 512], F32) for i in range(2)]
            sqc = sb(es, "sqc", [128, 512], F32)
            tp = [ps(es, f"tpc{i}", [128, 1024], BF16) for i in range(2)]
            p1 = [ps(es, f"p1{i}", [128, 512], F32) for i in range(2)]
            p2 = [ps(es, f"p2{i}", [128, 512], F32) for i in range(2)]

            P.dma("sp", gsgo[:], bc_rows(sgo_g, 0, 2048), r=[], w=["gsgo"], sem="c0")
            P.add("dve", lambda e: e.reduce_sum(out=ssg[:], in_=ssg_part[:], axis=AX.X), r=[], w=["ssg"])
            rstd_ops(c1[:], c2[:], ssg[:], 2048, "ssg", "rstd_sg", rstd_sg[:], "rsg")
            rstd_ops(c1[:], c2[:], ss_sb[:], 2048, "ss_sb", "rstd_sb", rstd_sb[:], "rsb")
            for j in range(NT):
                P.dma("sp", sg_t[:], sgu_scr[j * 128:(j + 1) * 128, :], r=[], w=["sg_t"], sem="sg_t")
                P.add("dve", lambda e: e.tensor_tensor(out=sg_g[:], in0=sg_t[:], in1=gsgo[:], op=ALU.mult), r=["sg_t", "gsgo"], w=["sg_g"])
                for g in range(4):
                    b_ = g % 2
                    for i in range(4):
                        kc = g * 4 + i
                        P.add("pe", lambda e, kc=kc, i=i, b_=b_: e.transpose(tp[b_][:, i * 128:(i + 1) * 128], sg_g[:, kc * 128:(kc + 1) * 128], ident[:]),
                              r=["sg_g", "ident"], w=[f"tpc{b_}"])
                    if g % 2 == 0:
                        P.add("act", lambda e, g=g, j=j, b_=b_: e.copy(out=sggT[:, g * 4:(g + 1) * 4, j * 128:(j + 1) * 128],
                                                                     in_=tp[b_][:, 0:512].rearrange("p (a b) -> p a b", a=4)),
                              r=[f"tpc{b_}"], w=["sggT"])
                    else:
                        P.add("dve", lambda e, g=g, j=j, b_=b_: e.tensor_copy(sggT[:, g * 4:(g + 1) * 4, j * 128:(j + 1) * 128],
                                                                            tp[b_][:, 0:512].rearrange("p (a b) -> p a b", a=4)),
                              r=[f"tpc{b_}"], w=["sggT"])
            wo_view = w_out.ap().rearrange("(kc p) n -> p kc n", p=128)

            def load_wo(ci):
                b_ = ci % 2
                P.dma("pool", wbuf[b_][:], wo_view[:, :, ci * 512:(ci + 1) * 512], r=[], w=[f"wbufc{b_}"], sem=f"wbufc{b_}")

            load_wo(0)
            it = 0
            for ci in range(8):
                if ci + 1 < 8:
                    load_wo(ci + 1)
                b_ = ci % 2
                for j in range(NT):
                    pb = it % 2
                    it += 1
                    P.dma("sp", xch[pb][:], x[j * 128:(j + 1) * 128, ci * 512:(ci + 1) * 512], r=[], w=[f"xch{pb}"], sem=f"xch{pb}")
                    for kc in range(16):
                        P.add("pe", lambda e, kc=kc, j=j, b_=b_, pb=pb: e.matmul(p1[pb][:], sbgT[:, kc, j * 128:(j + 1) * 128], wbuf[b_][:, kc, :],
                                                                               start=(kc == 0), stop=(kc == 15)),
                              r=["sbgT", f"wbufc{b_}"], w=[f"p1{pb}"])
                    for kc in range(16):
                        P.add("pe", lambda e, kc=kc, j=j, b_=b_, pb=pb: e.matmul(p2[pb][:], sggT[:, kc, j * 128:(j + 1) * 128], wbuf[b_][:, 16 + kc, :],
                                                                               start=(kc == 0), stop=(kc == 15)),
                              r=["sggT", f"wbufc{b_}"], w=[f"p2{pb}"])
                    P.add("dve", lambda e, j=j, pb=pb: e.scalar_tensor_tensor(out=hA[:], in0=p1[pb][:], scalar=rstd_sb[:, j:j + 1], in1=xch[pb][:],
                                                                            op0=ALU.mult, op1=ALU.add),
                          r=[f"p1{pb}", "rstd_sb", f"xch{pb}"], w=["hA"])
                    P.add("dve", lambda e, j=j, pb=pb: e.scalar_tensor_tensor(out=hch[pb][:], in0=p2[pb][:], scalar=rstd_sg[:, j:j + 1], in1=hA[:],
                                                                            op0=ALU.mult, op1=ALU.add),
                          r=[f"p2{pb}", "rstd_sg", "hA"], w=[f"hch{pb}"])
                    P.add("act", lambda e, j=j, ci=ci, pb=pb: e.activation(out=sqc[:], in_=hch[pb][:], func=AF.Square,
                                                                          accum_out=ssh_part[:, j, ci:ci + 1]),
                          r=[f"hch{pb}"], w=["sqc", "ssh_part"])
                    P.dma("sp", h_scr[j * 128:(j + 1) * 128, ci * 512:(ci + 1) * 512], hch[pb][:], r=[f"hch{pb}"], w=["h_scr"], sem="h_scr")
            P.emit()
        esBC.close()

    esP = ExitStack()
    h2T = sb(esP, "h2T", [128, 32, TOK], BF16)
    with ExitStack() as es:
        gffn = sb(es, "gffn", [128, D], F32)
        ht = sb(es, "ht", [128, D], F32)
        h2 = sb(es, "h2", [128, D], BF16)
        ssh = sb(es, "ssh", [128, NT], F32)
        rstd_h = sb(es, "rstd_h", [128, NT], F32)
        c1 = sb(es, "c2t1", [128, NT], F32)
        c2 = sb(es, "c2t2", [128, NT], F32)
        tp = [ps(es, f"tpd{i}", [128, 1024], BF16) for i in range(2)]
        P.dma("sp", gffn[:], bc_rows(ffn_g, 0, D), r=[], w=["gffn"], sem="c0")
        if only_DE:
            P.add("pool", lambda e: e.memset(ssh_part[:], 512.0), r=[], w=["ssh_part"])
            P.dma("sp", ident_f[:], ident_d[:, :], r=[], w=["ident_f"], sem="c0")
            P.add("dve", lambda e: e.tensor_copy(ident[:], ident_f[:]), r=["ident_f"], w=["ident"])
        P.add("dve", lambda e: e.reduce_sum(out=ssh[:], in_=ssh_part[:], axis=AX.X), r=["ssh_part"], w=["ssh"])
        rstd_ops(c1[:], c2[:], ssh[:], D, "ssh", "rstd_h", rstd_h[:], "rsh")
        for j in range(NT):
            P.dma("sp", ht[:], h_scr[j * 128:(j + 1) * 128, :], r=[], w=["ht"], sem="ht")
            P.add("dve", lambda e, j=j: e.scalar_tensor_tensor(out=h2[:], in0=ht[:], scalar=rstd_h[:, j:j + 1], in1=gffn[:],
                                                              op0=ALU.mult, op1=ALU.mult),
                  r=["ht", "rstd_h", "gffn"], w=["h2"])
            for g in range(8):
                b_ = g % 2
                for i in range(4):
                    dc = g * 4 + i
                    P.add("pe", lambda e, dc=dc, i=i, b_=b_: e.transpose(tp[b_][:, i * 128:(i + 1) * 128], h2[:, dc * 128:(dc + 1) * 128], ident[:]),
                          r=["h2", "ident"], w=[f"tpd{b_}"])
                if g % 2 == 0:
                    P.add("act", lambda e, g=g, j=j, b_=b_: e.copy(out=h2T[:, g * 4:(g + 1) * 4, j * 128:(j + 1) * 128],
                                                                 in_=tp[b_][:, 0:512].rearrange("p (a b) -> p a b", a=4)),
                          r=[f"tpd{b_}"], w=["h2T"])
                else:
                    P.add("dve", lambda e, g=g, j=j, b_=b_: e.tensor_copy(h2T[:, g * 4:(g + 1) * 4, j * 128:(j + 1) * 128],
                                                                        tp[b_][:, 0:512].rearrange("p (a b) -> p a b", a=4)),
                          r=[f"tpd{b_}"], w=["h2T"])
        if dbg:
            P.dma("sp", dbg_t["h"][:, :], h_scr[:, :], r=["h_scr"], w=["d8"], sem="dbg")
            P.dma("sp", dbg_t["h2T"][:, :], h2T[:].rearrange("p a b -> p (a b)"), r=["h2T"], w=["d9"], sem="dbg")
        P.emit()
    if stop_after == "C":
        esP.close()
        return finish()

    gs = sb(esP, "gs", [128, 5, NT, 8, 16], F32)
    A16s, B16s, I1s, I2s = gs[:, 0], gs[:, 1], gs[:, 2], gs[:, 3]
    th_s, negm_s, rZ_s = gs[:, 4, :, :, 0], gs[:, 4, :, :, 1], gs[:, 4, :, :, 2]
    NEG = -1.0e30
    with ExitStack() as es:
        wbuf = [sb(es, f"wbufd{i}", [128, 32, 512], BF16) for i in range(2)]
        skn = sb(es, "skn", [128, 8, 2, 256], F32)
        skh = sb(es, "skh", [128, 8, 2, 256], BF16)
        skl = sb(es, "skl", [128, 8, 2, 256], BF16)
        skT = sb(es, "skT", [128, 2, 8, 4, 128], BF16)
        qh = sb(es, "qh", [128, 512], BF16)
        ql = sb(es, "ql", [128, 512], BF16)
        qpT = sb(es, "qpT", [128, 2, 4, 128], BF16)
        tpb = ps(es, "tpb", [128, 1024], BF16)
        sc = sb(es, "sc", [128, 256], F32)
        tmpv = sb(es, "tmpv", [128, 128], F32)
        idx = sb(es, "idx", [128, 2, 16], U32)
        cand = sb(es, "cand", [128, 16, 16], F32)
        tmpc = sb(es, "tmpc", [128, 256], F32)
        C16 = sb(es, "C16", [128, 16], F32)
        e16 = sb(es, "e16", [128, 16], F32)
        Zt = sb(es, "Zt", [128, 1], F32)
        mmq = [ps(es, f"mmq{i}", [128, 512], F32) for i in range(2)]
        scp = ps(es, "scp", [128, 512], F32)

        P.dma("sp", skn[:], subk.ap().rearrange("h p n d -> n h p d"), r=[], w=["skn"], sem="c0")
        P.add("dve", lambda e: e.tensor_copy(skh[:], skn[:]), r=["skn"], w=["skh"])
        P.add("dve", lambda e: e.tensor_tensor(out=skl[:], in0=skn[:], in1=skh[:], op=ALU.subtract), r=["skn", "skh"], w=["skl"])
        for hl, sk_ in enumerate((skh, skl)):
            for hh in range(8):
                for pk in range(4):
                    p_, kc = pk // 2, pk % 2
                    P.add("pe", lambda e, hh=hh, pk=pk, p_=p_, kc=kc, sk_=sk_: e.transpose(tpb[:, pk * 128:(pk + 1) * 128],
                                                                                      sk_[:, hh, p_, kc * 128:(kc + 1) * 128], ident[:]),
                          r=["skh", "skl", "ident"], w=["tpb"])
                P.add("dve", lambda e, hh=hh, hl=hl: e.tensor_copy(skT[:, hl, hh, :, :], tpb[:, 0:512].rearrange("p (a b) -> p a b", a=4)),
                      r=["tpb"], w=["skT"])
        wq_view = w_q.ap().rearrange("(dc p) n -> p dc n", p=128)

        def load_wq(ci):
            b_ = ci % 2
            P.dma("pool", wbuf[b_][:], wq_view[:, :, ci * 512:(ci + 1) * 512], r=[], w=[f"wbufd{b_}"], sem=f"wbufd{b_}")

        load_wq(0)
        it = 0
        for hh in range(8):
            if hh + 1 < 8:
                load_wq(hh + 1)
            b_ = hh % 2
            for j in range(NT):
                pm = mmq[it % 2]
                pk_ = f"mmq{it % 2}"
                it += 1
                for dc in range(32):
                    P.add("pe", lambda e, dc=dc, j=j, b_=b_, pm=pm: e.matmul(pm[:], h2T[:, dc, j * 128:(j + 1) * 128], wbuf[b_][:, dc, :],
                                                                           start=(dc == 0), stop=(dc == 31)),
                          r=["h2T", f"wbufd{b_}"], w=[pk_])
                P.add("act", lambda e, pm=pm: e.copy(out=qh[:], in_=pm[:]), r=[pk_], w=["qh"])
                P.add("dve", lambda e, pm=pm: e.tensor_tensor(out=ql[:], in0=pm[:], in1=qh[:], op=ALU.subtract), r=[pk_, "qh"], w=["ql"])
                for hl, q_ in enumerate((qh, ql)):
                    for i in range(4):
                        P.add("pe", lambda e, i=i, hl=hl, q_=q_: e.transpose(tpb[:, (hl * 4 + i) * 128:(hl * 4 + i + 1) * 128],
                                                                           q_[:, i * 128:(i + 1) * 128], ident[:]),
                              r=["qh", "ql", "ident"], w=["tpb"])
                P.add("dve", lambda e: e.tensor_copy(qpT[:].rearrange("p a b c -> p (a b) c"), tpb[:].rearrange("p (a b) -> p a b", a=8)),
                      r=["tpb"], w=["qpT"])
                for p_ in range(2):
                    n_ = 0
                    for kc in range(2):
                        for (qa, ka) in ((0, 0), (0, 1), (1, 0)):
                            P.add("pe", lambda e, p_=p_, kc=kc, hh=hh, qa=qa, ka=ka, n_=n_: e.matmul(
                                scp[:, p_ * 128:(p_ + 1) * 128], qpT[:, qa, 2 * p_ + kc, :], skT[:, ka, hh, 2 * p_ + kc, :],
                                start=(n_ == 0), stop=(n_ == 5)), r=["qpT", "skT"], w=["scp"])
                            n_ += 1
                P.add("dve", lambda e: e.tensor_copy(sc[:], scp[:, 0:256]), r=["scp"], w=["sc"])
                for p_ in range(2):
                    T16 = (A16s if p_ == 0 else B16s)[:, j, hh, :]
                    Is = (I1s if p_ == 0 else I2s)[:, j, hh, :]
                    vv_ = sc[:, p_ * 128:(p_ + 1) * 128]
                    P.add("dve", lambda e, T16=T16, vv_=vv_: e.max(out=T16[:, 0:8], in_=vv_), r=["sc"], w=["gs"])
                    P.add("dve", lambda e, T16=T16, vv_=vv_: e.match_replace(out=tmpv[:], in_to_replace=T16[:, 0:8], in_values=vv_, imm_value=NEG),
                          r=["sc", "gs"], w=["tmpv"])
                    P.add("dve", lambda e, T16=T16: e.max(out=T16[:, 8:16], in_=tmpv[:]), r=["tmpv"], w=["gs"])
                    P.add("dve", lambda e, T16=T16, vv_=vv_, p_=p_: e.max_index(out=idx[:, p_, 0:8], in_max=T16[:, 0:8], in_values=vv_),
                          r=["sc", "gs"], w=["idx"])
                    P.add("dve", lambda e, T16=T16, vv_=vv_, p_=p_: e.max_index(out=idx[:, p_, 8:16], in_max=T16[:, 8:16], in_values=vv_),
                          r=["sc", "gs"], w=["idx"])
                    P.add("dve", lambda e, Is=Is, p_=p_: e.tensor_copy(Is, idx[:, p_, :]), r=["idx"], w=["gs"])
                a16 = A16s[:, j, hh, :]
                b16 = B16s[:, j, hh, :]
                P.add("dve", lambda e, a16=a16, b16=b16: e.tensor_tensor(out=cand[:], in0=a16.unsqueeze(2).broadcast_to([128, 16, 16]),
                                                                        in1=b16.unsqueeze(1).broadcast_to([128, 16, 16]), op=ALU.add),
                      r=["gs"], w=["cand"])
                cflat = cand[:].rearrange("p a b -> p (a b)")
                P.add("dve", lambda e, cflat=cflat: e.max(out=C16[:, 0:8], in_=cflat), r=["cand"], w=["C16"])
                P.add("dve", lambda e, cflat=cflat: e.match_replace(out=tmpc[:], in_to_replace=C16[:, 0:8], in_values=cflat, imm_value=NEG),
                      r=["cand", "C16"], w=["tmpc"])
                P.add("dve", lambda e: e.max(out=C16[:, 8:16], in_=tmpc[:]), r=["tmpc"], w=["C16"])
                P.add("dve", lambda e, j=j, hh=hh: e.tensor_copy(th_s[:, j, hh:hh + 1], C16[:, 15:16]), r=["C16"], w=["gs"])
                P.add("dve", lambda e, j=j, hh=hh: e.tensor_scalar(negm_s[:, j, hh:hh + 1], C16[:, 0:1], -1.0, None, ALU.mult), r=["C16"], w=["gs"])
                P.add("act", lambda e, j=j, hh=hh: e.activation(out=e16[:], in_=C16[:], func=AF.Exp, bias=negm_s[:, j, hh:hh + 1], scale=1.0,
                                                               accum_out=Zt[:, 0:1]), r=["C16", "gs"], w=["e16", "Zt"])
                P.add("dve", lambda e, j=j, hh=hh: e.reciprocal(rZ_s[:, j, hh:hh + 1], Zt[:, 0:1]), r=["Zt"], w=["gs"])
        if dbg:
            P.dma("sp", dbg_t["gs"][:, :], gs[:].rearrange("p a b c d -> p (a b c d)"), r=["gs"], w=["d10"], sem="dbg")
        P.emit()
    if stop_after == "D":
        esP.close()
        return finish()

    with ExitStack() as es:
        cand8 = sb(es, "cand8", [128, 8, 16, 16], F32)
        Ee = sb(es, "Ee", [128, 256], F32)
        Cgm = sb(es, "Cgm", [128, 256], F32)
        Cg = sb(es, "Cg", [128, 16, 8, 16], BF16)
        IT = sb(es, "IT", [128, 2, 128], F32)
        iota_i = sb(es, "iota_i", [128, 128], mybir.dt.int32)
        iota_f = sb(es, "iota_f", [128, 128], F32)
        bm_f = sb(es, "bm_f", [128, 8], F32)
        bmask = sb(es, "bmask", [128, 8], BF16)
        BD = sb(es, "BD", [128, 64, 8, 16], BF16)
        Xo = sb(es, "Xo", [128, 64, 128], BF16)
        Yo = sb(es, "Yo", [128, 64, 128], BF16)
        Wsb = sb(es, "Wsb", [128, 64, 128], BF16)
        Gsb = sb(es, "Gsb", [128, 128, 128], BF16)
        tI = ps(es, "tI", [128, 1024], BF16)
        Ib = sb(es, "Ib", [128, 2, 128], BF16)
        tcA = ps(es, "tcA", [128, 1024], BF16)
        tcB = ps(es, "tcB", [128, 1024], BF16)
        Wps = [ps(es, f"Wps{i}", [128, 512], F32) for i in range(2)]
        Gps = [ps(es, f"Gps{i}", [128, 512], F32) for i in range(2)]

        P.add("pool", lambda e: e.iota(iota_i[:], pattern=[[1, 128]], base=0, channel_multiplier=0), r=[], w=["iota_i"])
        P.add("dve", lambda e: e.tensor_copy(iota_f[:], iota_i[:]), r=["iota_i"], w=["iota_f"])
        P.add("pool", lambda e: e.memset(bm_f[:], 1.0), r=[], w=["bm_f"])
        P.add("pool", lambda e: e.affine_select(out=bm_f[:], in_=bm_f[:], pattern=[[-16, 8]], compare_op=ALU.is_ge, fill=0.0,
                                                base=0, channel_multiplier=1), r=["bm_f"], w=["bm_f"])
        P.add("pool", lambda e: e.affine_select(out=bm_f[:], in_=bm_f[:], pattern=[[16, 8]], compare_op=ALU.is_ge, fill=0.0,
                                                base=15, channel_multiplier=-1), r=["bm_f"], w=["bm_f"])
        P.add("dve", lambda e: e.tensor_copy(bmask[:], bm_f[:]), r=["bm_f"], w=["bmask"])
        G_view = G_scr.ap().rearrange("(c i) t -> i c t", i=128)
        ev = 0
        for j in range(NT):
            P.add("dve", lambda e, j=j: e.tensor_tensor(out=cand8[:], in0=A16s[:, j, :, :].unsqueeze(3).broadcast_to([128, 8, 16, 16]),
                                                       in1=B16s[:, j, :, :].unsqueeze(2).broadcast_to([128, 8, 16, 16]), op=ALU.add),
                  r=[], w=["cand8"])
            for hh in range(8):
                ch = cand8[:, hh, :, :].rearrange("p a b -> p (a b)")
                P.add("act", lambda e, ch=ch, j=j, hh=hh: e.activation(out=Ee[:], in_=ch, func=AF.Exp, bias=negm_s[:, j, hh:hh + 1], scale=1.0),
                      r=["cand8"], w=["Ee"])
                P.add("dve", lambda e, ch=ch, j=j, hh=hh: e.scalar_tensor_tensor(out=Cgm[:], in0=ch, scalar=th_s[:, j, hh:hh + 1], in1=Ee[:],
                                                                               op0=ALU.is_ge, op1=ALU.mult),
                      r=["cand8", "Ee"], w=["Cgm"])
                P.add("dve", lambda e, j=j, hh=hh: e.tensor_scalar(Cg[:, :, hh, :], Cgm[:].rearrange("p (a b) -> p a b", a=16),
                                                                  rZ_s[:, j, hh:hh + 1], None, ALU.mult),
                      r=["Cgm"], w=["Cg"])
            P.add("dve", lambda e, j=j: e.tensor_copy(Ib[:, 0, :], I1s[:, j, :, :].rearrange("p a b -> p (a b)")), r=[], w=["Ib"])
            P.add("dve", lambda e, j=j: e.tensor_copy(Ib[:, 1, :], I2s[:, j, :, :].rearrange("p a b -> p (a b)")), r=[], w=["Ib"])
            P.add("pe", lambda e: e.transpose(tI[:, 0:128], Ib[:, 0, :], ident[:]), r=["Ib", "ident"], w=["tI"])
            P.add("pe", lambda e: e.transpose(tI[:, 128:256], Ib[:, 1, :], ident[:]), r=["Ib", "ident"], w=["tI"])
            P.add("dve", lambda e: e.tensor_copy(IT[:], tI[:, 0:256].rearrange("p (a b) -> p a b", a=2)), r=["tI"], w=["IT"])
            for r1 in range(16):
                tc_ = tcA if r1 < 8 else tcB
                P.add("pe", lambda e, r1=r1, tc_=tc_: e.transpose(tc_[:, (r1 % 8) * 128:(r1 % 8 + 1) * 128],
                                                                Cg[:, r1, :, :].rearrange("p a b -> p (a b)"), ident[:]),
                      r=["Cg", "ident"], w=["tcA" if r1 < 8 else "tcB"])
            for hf in range(2):
                t0 = 64 * hf
                for q_, tc_ in enumerate((tcA, tcB)):
                    src_ = tc_[:, :].rearrange("p (r t) -> p t r", r=8)[:, t0:t0 + 64, :].unsqueeze(2).broadcast_to([128, 64, 8, 8])
                    msk_ = bmask[:, :].unsqueeze(1).unsqueeze(3).broadcast_to([128, 64, 8, 8])
                    P.add("dve", lambda e, q_=q_, src_=src_, msk_=msk_: e.tensor_tensor(out=BD[:, :, :, 8 * q_:8 * q_ + 8], in0=src_, in1=msk_, op=ALU.mult),
                          r=["tcA" if q_ == 0 else "tcB", "bmask"], w=["BD"])
                for q_, dst_ in enumerate((Xo, Yo)):
                    P.add("dve", lambda e, q_=q_, dst_=dst_, t0=t0: e.tensor_tensor(
                        out=dst_[:], in0=iota_f[:, :].unsqueeze(1).broadcast_to([128, 64, 128]),
                        in1=IT[:, q_, t0:t0 + 64].unsqueeze(2).broadcast_to([128, 64, 128]), op=ALU.is_equal),
                        r=["iota_f", "IT"], w=["Xo" if q_ == 0 else "Yo"])
                for t4 in range(16):
                    wb = ev % 2
                    ev += 1
                    for tt in range(4):
                        t = t4 * 4 + tt
                        P.add("pe", lambda e, t=t, tt=tt, wb=wb: e.matmul(Wps[wb][:, tt * 128:(tt + 1) * 128],
                                                                        BD[:, t, :, :].rearrange("p a b -> p (a b)"), Yo[:, t, :],
                                                                        start=True, stop=True),
                              r=["BD", "Yo"], w=[f"Wps{wb}"])
                    if t4 % 2 == 0:
                        P.add("act", lambda e, t4=t4, wb=wb: e.copy(out=Wsb[:, t4 * 4:t4 * 4 + 4, :], in_=Wps[wb][:].rearrange("p (a b) -> p a b", a=4)),
                              r=[f"Wps{wb}"], w=["Wsb"])
                    else:
                        P.add("dve", lambda e, t4=t4, wb=wb: e.tensor_copy(Wsb[:, t4 * 4:t4 * 4 + 4, :], Wps[wb][:].rearrange("p (a b) -> p a b", a=4)),
                              r=[f"Wps{wb}"], w=["Wsb"])
                for t4 in range(16):
                    gb = ev % 2
                    ev += 1
                    for tt in range(4):
                        t = t4 * 4 + tt
                        P.add("pe", lambda e, t=t, tt=tt, gb=gb: e.matmul(Gps[gb][:, tt * 128:(tt + 1) * 128], Xo[:, t, :], Wsb[:, t, :],
                                                                        start=True, stop=True),
                              r=["Xo", "Wsb"], w=[f"Gps{gb}"])
                    tg = t0 + t4 * 4
                    dst_ = Gsb[:, :, tg:tg + 4].rearrange("p c t -> p t c")
                    if t4 % 2 == 0:
                        P.add("act", lambda e, dst_=dst_, gb=gb: e.copy(out=dst_, in_=Gps[gb][:].rearrange("p (a b) -> p a b", a=4)),
                              r=[f"Gps{gb}"], w=["Gsb"])
                    else:
                        P.add("dve", lambda e, dst_=dst_, gb=gb: e.tensor_copy(dst_, Gps[gb][:].rearrange("p (a b) -> p a b", a=4)),
                              r=[f"Gps{gb}"], w=["Gsb"])
            for cg in range(8):
                P.dma("sp", G_view[:, cg * 16:(cg + 1) * 16, j * 128:(j + 1) * 128], Gsb[:, cg * 16:(cg + 1) * 16, :],
                      r=["Gsb"], w=["G_scr"], sem="G_scr")
        if dbg:
            P.dma("sp", dbg_t["G"][:, :], G_scr[:, :], r=["G_scr"], w=["d11"], sem="dbg")
        P.emit()
    if stop_after == "E":
        esP.close()
        return finish()

    with ExitStack() as es:
        Uc = [sb(es, f"Uc{i}", [128, D], BF16) for i in range(2)]
        UT = [sb(es, f"UT{i}", [128, 32, 128], BF16) for i in range(2)]
        Gc = [sb(es, f"Gc{i}", [128, TOK], BF16) for i in range(2)]
        gel = [sb(es, f"gel{i}", [128, 512], F32) for i in range(2)]
        cfT = [sb(es, f"cfT{i}", [128, TOK], BF16) for i in range(2)]
        tpu = [ps(es, f"tpu{i}", [128, 1024], BF16) for i in range(2)]
        aps = [ps(es, f"aps{i}", [128, 512], F32) for i in range(4)]
        pu_v = pu.ap().rearrange("(i c) d -> i c d", c=128)

        def load_u(c):
            b_ = c % 2
            P.dma("pool", Uc[b_][:], pu_v[:, c, :], r=[], w=[f"Uc{b_}"], sem=f"Uc{b_}")
            P.dma("sp", Gc[b_][:], G_scr[c * 128:(c + 1) * 128, :], r=[], w=[f"Gc{b_}"], sem=f"Gc{b_}")

        load_u(0)
        tq = 0
        aq = 0
        for c in range(128):
            if c + 1 < 128:
                load_u(c + 1)
            b_ = c % 2
            for g in range(4):
                tb = tq % 2
                tq += 1
                for i in range(8):
                    dc = g * 8 + i
                    P.add("pe", lambda e, dc=dc, i=i, tb=tb, b_=b_: e.transpose(tpu[tb][:, i * 128:(i + 1) * 128], Uc[b_][:, dc * 128:(dc + 1) * 128], ident[:]),
                          r=[f"Uc{b_}", "ident"], w=[f"tpu{tb}"])
                if g % 2 == 0:
                    P.add("act", lambda e, g=g, tb=tb, b_=b_: e.copy(out=UT[b_][:, g * 8:(g + 1) * 8, :], in_=tpu[tb][:].rearrange("p (a b) -> p a b", a=8)),
                          r=[f"tpu{tb}"], w=[f"UT{b_}"])
                else:
                    P.add("dve", lambda e, g=g, tb=tb, b_=b_: e.tensor_copy(UT[b_][:, g * 8:(g + 1) * 8, :], tpu[tb][:].rearrange("p (a b) -> p a b", a=8)),
                          r=[f"tpu{tb}"], w=[f"UT{b_}"])
            for hf in range(2):
                ab = aq % 4
                gb = aq % 2
                aq += 1
                for dc in range(32):
                    P.add("pe", lambda e, dc=dc, hf=hf, ab=ab, b_=b_: e.matmul(aps[ab][:], UT[b_][:, dc, :], h2T[:, dc, hf * 512:(hf + 1) * 512],
                                                                             start=(dc == 0), stop=(dc == 31)),
                          r=[f"UT{b_}", "h2T"], w=[f"aps{ab}"])
                P.add("act", lambda e, ab=ab, gb=gb: e.activation(out=gel[gb][:], in_=aps[ab][:], func=AF.Gelu_apprx_tanh),
                      r=[f"aps{ab}"], w=[f"gel{gb}"])
                P.add("dve", lambda e, gb=gb, hf=hf, b_=b_: e.tensor_tensor(out=cfT[b_][:, hf * 512:(hf + 1) * 512], in0=gel[gb][:],
                                                                          in1=Gc[b_][:, hf * 512:(hf + 1) * 512], op=ALU.mult),
                      r=[f"gel{gb}", f"Gc{b_}"], w=[f"cfT{b_}"])
            P.dma("sp", coef_scr[c * 128:(c + 1) * 128, :], cfT[b_][:], r=[f"cfT{b_}"], w=["coef_scr"], sem="coef_scr")
        P.emit()
    esP.close()
    if stop_after == "F":
        return finish()

    with ExitStack() as es:
        NB = 4
        Vg = [sb(es, f"Vg{i}", [128, 4, 512], BF16) for i in range(NB)]
        Cf = [sb(es, f"Cf{i}", [128, 4, TOK], BF16) for i in range(NB)]
        hc = [sb(es, f"hc{i}", [128, 512], F32) for i in range(2)]
        yo = [sb(es, f"yo{i}", [128, 512], F32) for i in range(2)]
        ob = [ps(es, f"ob{i}", [128, 512], F32) for i in range(8)]
        pv_v = pv.ap().rearrange("(i c) d -> i c d", c=128)
        coef_v = coef_scr.ap().rearrange("(c i) t -> i c t", i=128)
        loads = [(dr, cg) for dr in range(8) for cg in range(32)]

        def load_g(k):
            dr, cg = loads[k]
            bf_ = k % NB
            P.dma("pool", Vg[bf_][:], pv_v[:, 4 * cg:4 * cg + 4, dr * 512:(dr + 1) * 512], r=[], w=[f"Vg{bf_}"], sem=f"Vg{bf_}")
            P.dma("sp", Cf[bf_][:], coef_v[:, 4 * cg:4 * cg + 4, :], r=["coef_scr"], w=[f"Cf{bf_}"], sem=f"Cf{bf_}")

        for k in range(NB - 1):
            load_g(k)
        k = 0
        eo = 0
        for dr in range(8):
            for cg in range(32):
                if k + NB - 1 < len(loads):
                    load_g(k + NB - 1)
                bf_ = k % NB
                k += 1
                for cc in range(4):
                    for j in range(NT):
                        first = (cg == 0 and cc == 0)
                        last = (cg == 31 and cc == 3)
                        P.add("pe", lambda e, cc=cc, j=j, bf_=bf_, first=first, last=last: e.matmul(
                            ob[j][:], Cf[bf_][:, cc, j * 128:(j + 1) * 128], Vg[bf_][:, cc, :], start=first, stop=last),
                            r=[f"Cf{bf_}", f"Vg{bf_}"], w=[f"ob{j}"])
            for j in range(NT):
                eb = eo % 2
                eo += 1
                P.dma("sp", hc[eb][:], h_scr[j * 128:(j + 1) * 128, dr * 512:(dr + 1) * 512], r=[], w=[f"hc{eb}"], sem=f"hc{eb}")
                P.add("dve", lambda e, j=j, eb=eb: e.tensor_tensor(out=yo[eb][:], in0=ob[j][:], in1=hc[eb][:], op=ALU.add),
                      r=[f"ob{j}", f"hc{eb}"], w=[f"yo{eb}"])
                P.dma("sp", y[j * 128:(j + 1) * 128, dr * 512:(dr + 1) * 512], yo[eb][:], r=[f"yo{eb}"], w=["y"], sem="y")
        P.emit()
    return nc


def make_in_maps(inputs):
    x = np.asarray(inputs["x"], dtype=np.float32).reshape(8192, D)
    xb = x.reshape(8, NCORES, 128, D)
    ident = np.eye(128, dtype=np.float32)
    shared = {
        "w_in": None,
        "mix_norm_g": np.asarray(inputs["mix_norm_g"], np.float32).reshape(1, D),
        "q_norm_g": np.asarray(inputs["q_norm_g"], np.float32).reshape(1, 128),
        "k_norm_g": np.asarray(inputs["k_norm_g"], np.float32).reshape(1, 128),
        "sgu_v_norm_g": np.asarray(inputs["sgu_v_norm_g"], np.float32).reshape(1, 2048),
        "sgu_w": np.ascontiguousarray(np.asarray(inputs["sgu_w"], np.float32)[0]),
        "sgu_b": np.ascontiguousarray(np.asarray(inputs["sgu_b"], np.float32)[0]),
        "sb_out_norm_g": np.asarray(inputs["sb_out_norm_g"], np.float32).reshape(1, 2048),
        "sgu_out_norm_g": np.asarray(inputs["sgu_out_norm_g"], np.float32).reshape(1, 2048),
        "w_out": None,
        "ffn_norm_g": np.asarray(inputs["ffn_norm_g"], np.float32).reshape(1, D),
        "peer_w_q": None,
        "peer_sub_keys": np.ascontiguousarray(np.asarray(inputs["peer_sub_keys"], np.float32)[0]),
        "peer_u": None,
        "peer_v": None,
        "ident": ident,
    }
    maps = []
    s = np.arange(128)[:, None, None]
    m = np.arange(8)[None, :, None]
    t = np.arange(128)[None, None, :]
    for c in range(NCORES):
        d = dict(shared)
        d["x"] = np.ascontiguousarray(xb[:, c].reshape(TOK, D))
        for nm in ("w_in", "w_out", "peer_w_q", "peer_u", "peer_v"):
            full = np.asarray(inputs[nm], np.float32)[0]
            n = full.shape[0] // NCORES
            d[nm] = np.ascontiguousarray(full[c * n:(c + 1) * n])
        d["amask"] = np.ascontiguousarray((128 * m + s < 128 * c + t).astype(np.float32))
        maps.append(d)
    return maps


_CACHE = {}


def kernel(**inputs):
    maps = make_in_maps(inputs)
    if "nc" not in _CACHE:
        _CACHE["nc"] = build()
    nc = _CACHE["nc"]
    res = run_bass_kernel_spmd(nc, maps, core_ids=list(range(NCORES)))
    out = np.empty((8, NCORES, 128, D), np.float32)
    for c in range(NCORES):
        out[:, c] = np.asarray(res.results[c]["y"], dtype=np.float32).reshape(8, 128, D)
    return out.reshape(1, 8192, D)
```
